# Optimizing a Trainium2 kernel written in Bass

```python
import jax, jax.numpy as jnp
from jax import lax
import numpy as np

D_MODEL = 1024
BATCH = 2
SEQ = 8192
DEPTH = 1

CHUNK = 64
MIX_WIDTH = D_MODEL
RET_WIDTH = MIX_WIDTH // 2
RWKV_WIDTH = MIX_WIDTH - RET_WIDTH
RET_HEADS = 4
RET_HEAD_DIM = RET_WIDTH // RET_HEADS
RWKV_HEAD_DIM = 64
RWKV_HEADS = RWKV_WIDTH // RWKV_HEAD_DIM
DECAY_LORA = 64
AAA_LORA = 64
GATE_LORA = 128
RWKV_COLS = 3 * RWKV_WIDTH + DECAY_LORA + AAA_LORA + GATE_LORA
IN_COLS = 4 * RET_WIDTH + RWKV_COLS
N_GROUPS = 4
EXPERTS_PER_GROUP = 8
N_EXPERTS = N_GROUPS * EXPERTS_PER_GROUP
TOP_K = 2
D_EXPERT = 512
MOE_BLOCK = 128
ROPE_BASE = 10000.0
NORM_EPS = 1e-6
RET_GN_EPS = 1e-5
RWKV_GN_EPS = 64e-5

kernel_name = 'hymba_retnet_rwkv7_hiermoe_block'


def rms_norm(x, gain):
    xf = x.astype(jnp.float32)
    y = xf * lax.rsqrt(jnp.mean(xf * xf, axis=-1, keepdims=True) + NORM_EPS)
    return (y * gain.astype(jnp.float32)).astype(x.dtype)


def head_norm(y, gain, eps):
    mu = jnp.mean(y, axis=-1, keepdims=True)
    yc = y - mu
    var = jnp.mean(yc * yc, axis=-1, keepdims=True)
    return yc * lax.rsqrt(var + eps) * gain.astype(jnp.float32)


def rotary(x):
    S, d = x.shape[1], x.shape[-1]
    half = d // 2
    inv = ROPE_BASE ** (-jnp.arange(half, dtype=jnp.float32) / half)
    ang = jnp.arange(S, dtype=jnp.float32)[:, None] * inv[None, :]
    cos = jnp.cos(ang)[None, :, None, :]
    sin = jnp.sin(ang)[None, :, None, :]
    x1, x2 = x[..., :half], x[..., half:]
    return jnp.concatenate([x1 * cos - x2 * sin, x1 * sin + x2 * cos], axis=-1)


def retention(q, k, v):
    B, S, H, d = q.shape
    NC = S // CHUNK
    log_g = jnp.log(1.0 - jnp.exp2(-5.0 - jnp.arange(H, dtype=jnp.float32)))
    idx = jnp.arange(CHUNK, dtype=jnp.float32)
    intra_decay = jnp.exp(log_g[:, None, None] * jnp.abs(idx[:, None] - idx[None, :]))
    k_decay = jnp.exp(log_g[:, None] * (CHUNK - 1.0 - idx)[None, :])
    q_decay = jnp.exp(log_g[:, None] * (idx + 1.0)[None, :])
    chunk_decay = jnp.exp(log_g * CHUNK)
    qc = q.reshape(B, NC, CHUNK, H, d)
    kc = k.reshape(B, NC, CHUNK, H, d) * (d ** -0.5)
    vc = v.reshape(B, NC, CHUNK, H, d)
    scores = jnp.einsum('bnchd,bnmhd->bnhcm', qc, kc) * intra_decay[None, None]
    intra = jnp.einsum('bnhcm,bnmhe->bnche', scores, vc)
    kv = jnp.einsum('bnmhd,hm,bnmhe->bnhde', kc, k_decay, vc)

    def step(state, kv_n):
        return state * chunk_decay[None, :, None, None] + kv_n, state

    _, prev = lax.scan(step, jnp.zeros((B, H, d, d), jnp.float32), jnp.moveaxis(kv, 1, 0))
    prev = jnp.moveaxis(prev, 0, 1)
    cross = jnp.einsum('bnchd,hc,bnhde->bnche', qc, q_decay, prev)
    return (intra + cross).reshape(B, S, H, d)


def token_shift_lerp(feat, mu):
    prev = jnp.concatenate([jnp.zeros_like(feat[:, :1]), feat[:, :-1]], axis=1)
    return feat + (prev - feat) * mu


def rwkv7_time_mix(feats, mu, w0, w_up, a0, a_up, g_up, k_k, k_a, r_k, gn_gain):
    B, S, _ = feats.shape
    H, N, W = RWKV_HEADS, RWKV_HEAD_DIM, RWKV_WIDTH
    f = token_shift_lerp(feats.astype(jnp.float32), mu.astype(jnp.float32))
    r = f[..., 0:W]
    k = f[..., W:2 * W]
    v = f[..., 2 * W:3 * W]
    o = 3 * W
    w_lo = f[..., o:o + DECAY_LORA]
    a_lo = f[..., o + DECAY_LORA:o + DECAY_LORA + AAA_LORA]
    g_lo = f[..., o + DECAY_LORA + AAA_LORA:]
    w = -jax.nn.softplus(-(w0 + jnp.tanh(w_lo) @ w_up)) - 0.5
    decay = jnp.exp(-jnp.exp(w))
    a = jax.nn.sigmoid(a0 + a_lo @ a_up)
    g = jax.nn.sigmoid(g_lo) @ g_up
    kk = (k * k_k).reshape(B, S, H, N)
    kk = kk / jnp.maximum(jnp.sqrt(jnp.sum(kk * kk, axis=-1, keepdims=True)), 1e-12)
    k = k * (1.0 + (a - 1.0) * k_a)
    rh = r.reshape(B, S, H, N)
    kh = k.reshape(B, S, H, N)
    vh = v.reshape(B, S, H, N)
    ah = a.reshape(B, S, H, N)
    dh = decay.reshape(B, S, H, N)
    a_vec = -kk
    b_vec = kk * ah

    def step(state, inp):
        r_t, k_t, v_t, w_t, a_t, b_t = inp
        sa = jnp.einsum('bhvk,bhk->bhv', state, a_t)
        state = state * w_t[:, :, None, :] + sa[..., None] * b_t[:, :, None, :] + v_t[..., None] * k_t[:, :, None, :]
        return state, jnp.einsum('bhvk,bhk->bhv', state, r_t)

    xs = tuple(jnp.moveaxis(t, 1, 0) for t in (rh, kh, vh, dh, a_vec, b_vec))
    _, y = lax.scan(step, jnp.zeros((B, H, N, N), jnp.float32), xs)
    y = jnp.moveaxis(y, 0, 1)
    y = head_norm(y, gn_gain.reshape(H, N), RWKV_GN_EPS)
    bonus = jnp.sum(rh * kh * r_k[None, None].astype(jnp.float32), axis=-1, keepdims=True) * vh
    return (y + bonus).reshape(B, S, W) * g


def hier_moe(xn, w_route_group, b_route_group, w_route_expert, b_route_expert, w_gate, w_up, w_down):
    B, S, D = xn.shape
    T = B * S
    xt = xn.reshape(T, D)
    group_logits = (xt @ w_route_group).astype(jnp.float32) + b_route_group.astype(jnp.float32)
    group_prob = jax.nn.softmax(group_logits, axis=-1)
    g_idx = jnp.argmax(group_logits, axis=-1).astype(jnp.int32)
    g_p = jnp.take_along_axis(group_prob, g_idx[:, None], axis=1)
    exp_logits = ((xt @ w_route_expert).astype(jnp.float32) + b_route_expert.astype(jnp.float32)).reshape(T, N_GROUPS, EXPERTS_PER_GROUP)
    in_group = jnp.take_along_axis(exp_logits, g_idx[:, None, None], axis=1)[:, 0]
    top_v, top_i = lax.top_k(in_group, TOP_K)
    top_w = jax.nn.softmax(top_v, axis=-1) * g_p
    expert_id = (g_idx[:, None] * EXPERTS_PER_GROUP + top_i.astype(jnp.int32)).reshape(-1)
    token_id = jnp.repeat(jnp.arange(T, dtype=jnp.int32), TOP_K)
    weight = top_w.reshape(-1)
    A = T * TOP_K
    order = jnp.argsort(expert_id)
    se, st, sw = expert_id[order], token_id[order], weight[order]
    counts = jax.ops.segment_sum(jnp.ones((A,), jnp.int32), expert_id, num_segments=N_EXPERTS)
    starts = jnp.cumsum(counts) - counts
    padded = (counts + MOE_BLOCK - 1) // MOE_BLOCK * MOE_BLOCK
    pad_ends = jnp.cumsum(padded)
    pad_starts = pad_ends - padded
    dest = pad_starts[se] + jnp.arange(A, dtype=jnp.int32) - starts[se]
    P = ((A + MOE_BLOCK - 1) // MOE_BLOCK + N_EXPERTS) * MOE_BLOCK
    NB = P // MOE_BLOCK
    slot_tok = jnp.full((P,), T, jnp.int32).at[dest].set(st)
    slot_w = jnp.zeros((P,), jnp.float32).at[dest].set(sw)
    block_expert = jnp.clip(jnp.searchsorted(pad_ends, jnp.arange(NB, dtype=jnp.int32) * MOE_BLOCK, side='right'), 0, N_EXPERTS - 1)
    x_pad = jnp.concatenate([xt, jnp.zeros((1, D), xt.dtype)], axis=0)
    xb = x_pad[slot_tok].reshape(NB, MOE_BLOCK, D)

    def expert_block(args):
        xblk, e = args
        hdn = jax.nn.silu(xblk @ w_gate[e]) * (xblk @ w_up[e])
        return hdn @ w_down[e]

    yb = lax.map(expert_block, (xb, block_expert)).reshape(P, D)
    y = jax.ops.segment_sum(yb.astype(jnp.float32) * slot_w[:, None], slot_tok, num_segments=T + 1)[:T]
    return y.reshape(B, S, D).astype(xn.dtype)


def setup_inputs(seed: int = 0) -> dict:
    key = jax.random.key(seed)
    ks = jax.random.split(key, 24)
    L, D = DEPTH, D_MODEL
    nrm = lambda k, shape, s: jax.random.normal(k, shape, jnp.float32) * s
    return {
        'x': nrm(ks[0], (BATCH, SEQ, D), 1.0),
        'norm1_gain': 1.0 + nrm(ks[1], (L, D), 0.02),
        'w_in': nrm(ks[2], (L, D, IN_COLS), D ** -0.5),
        'ret_gn_gain': 1.0 + nrm(ks[3], (L, RET_WIDTH), 0.02),
        'rwkv_mu': jax.random.uniform(ks[4], (L, RWKV_COLS), jnp.float32),
        'rwkv_w0': nrm(ks[5], (L, RWKV_WIDTH), 0.5) + 0.5,
        'rwkv_w_up': nrm(ks[6], (L, DECAY_LORA, RWKV_WIDTH), 0.5 * DECAY_LORA ** -0.5),
        'rwkv_a0': nrm(ks[7], (L, RWKV_WIDTH), 0.5),
        'rwkv_a_up': nrm(ks[8], (L, AAA_LORA, RWKV_WIDTH), 0.5 * AAA_LORA ** -0.5),
        'rwkv_g_up': nrm(ks[9], (L, GATE_LORA, RWKV_WIDTH), GATE_LORA ** -0.5),
        'rwkv_k_k': 0.85 + nrm(ks[10], (L, RWKV_WIDTH), 0.05),
        'rwkv_k_a': 1.0 + nrm(ks[11], (L, RWKV_WIDTH), 0.05),
        'rwkv_r_k': nrm(ks[12], (L, RWKV_HEADS, RWKV_HEAD_DIM), 0.1),
        'rwkv_gn_gain': 1.0 + nrm(ks[13], (L, RWKV_WIDTH), 0.02),
        'w_out': nrm(ks[14], (L, MIX_WIDTH, D), MIX_WIDTH ** -0.5),
        'norm2_gain': 1.0 + nrm(ks[15], (L, D), 0.02),
        'w_route_group': nrm(ks[16], (L, D, N_GROUPS), D ** -0.5),
        'b_route_group': nrm(ks[17], (L, N_GROUPS), 0.01),
        'w_route_expert': nrm(ks[18], (L, D, N_EXPERTS), D ** -0.5),
        'b_route_expert': nrm(ks[19], (L, N_EXPERTS), 0.01),
        'w_gate': nrm(ks[20], (L, N_EXPERTS, D, D_EXPERT), D ** -0.5),
        'w_up': nrm(ks[21], (L, N_EXPERTS, D, D_EXPERT), D ** -0.5),
        'w_down': nrm(ks[22], (L, N_EXPERTS, D_EXPERT, D), D_EXPERT ** -0.5),
        'final_norm_gain': 1.0 + nrm(ks[23], (D,), 0.02),
    }


def reference(x, norm1_gain, w_in, ret_gn_gain, rwkv_mu, rwkv_w0, rwkv_w_up, rwkv_a0, rwkv_a_up, rwkv_g_up, rwkv_k_k, rwkv_k_a, rwkv_r_k, rwkv_gn_gain, w_out, norm2_gain, w_route_group, b_route_group, w_route_expert, b_route_expert, w_gate, w_up, w_down, final_norm_gain):
    B, S, _ = x.shape
    h = x
    for l in range(DEPTH):
        xn = rms_norm(h, norm1_gain[l])
        proj = xn @ w_in[l]
        rp = proj[..., :4 * RET_WIDTH].astype(jnp.float32)
        q = rotary(rp[..., 0:RET_WIDTH].reshape(B, S, RET_HEADS, RET_HEAD_DIM))
        k = rotary(rp[..., RET_WIDTH:2 * RET_WIDTH].reshape(B, S, RET_HEADS, RET_HEAD_DIM))
        v = rp[..., 2 * RET_WIDTH:3 * RET_WIDTH].reshape(B, S, RET_HEADS, RET_HEAD_DIM)
        ret = head_norm(retention(q, k, v), ret_gn_gain[l].reshape(RET_HEADS, RET_HEAD_DIM), RET_GN_EPS)
        ret = jax.nn.silu(rp[..., 3 * RET_WIDTH:]) * ret.reshape(B, S, RET_WIDTH)
        rw = rwkv7_time_mix(proj[..., 4 * RET_WIDTH:], rwkv_mu[l], rwkv_w0[l], rwkv_w_up[l], rwkv_a0[l], rwkv_a_up[l], rwkv_g_up[l], rwkv_k_k[l], rwkv_k_a[l], rwkv_r_k[l], rwkv_gn_gain[l])
        mixed = jnp.concatenate([ret, rw], axis=-1).astype(x.dtype)
        h = h + mixed @ w_out[l]
        h = h + hier_moe(rms_norm(h, norm2_gain[l]), w_route_group[l], b_route_group[l], w_route_expert[l], b_route_expert[l], w_gate[l], w_up[l], w_down[l])
    return rms_norm(h, final_norm_gain)
```

```python
import contextlib
import os
import math
import numpy as np
import concourse.bass as bass
import concourse.mybir as mybir
from concourse.bass_utils import run_bass_kernel_spmd

F32 = mybir.dt.float32
BF16 = mybir.dt.bfloat16
I32 = mybir.dt.int32
AF = mybir.ActivationFunctionType
ALU = mybir.AluOpType
AX = mybir.AxisListType

NCORE = 8
SEG = 2048
NT = 16
D = 1024
CAP = 256
NEXP = 32
GAM = [1.0 - 2.0 ** (-5 - h) for h in range(4)]
EM05 = math.exp(-0.5)


class Buf:
    __slots__ = ("name", "last_w", "readers", "sem", "ndma", "psum")

    def __init__(self, name):
        self.name = name
        self.last_w = None
        self.readers = []
        self.sem = None
        self.ndma = 0
        self.psum = name.startswith("ps")


class FW:
    ENGS = ("pe", "act", "dve", "pool", "sp")

    def __init__(self, nc, ctx):
        self.nc = nc
        self.ctx = ctx
        self.items = {e: [] for e in self.ENGS}
        self.count = {e: 0 for e in self.ENGS}
        self.waited = {e: {} for e in self.ENGS}
        self.esem = {e: ctx.enter_context(nc.semaphore("es_" + e)) for e in self.ENGS}
        self.sems = {}
        for e in self.ENGS:
            self.sems[("e", e)] = self.esem[e]
        self.dmabufs = []
        self.nbuf = 0

    def buf(self, name=None):
        self.nbuf += 1
        return Buf(name or "b%d" % self.nbuf)

    def _deps(self, reads, writes):
        deps = []
        for b in reads:
            if b.last_w is not None:
                deps.append(b.last_w)
        for b in writes:
            if b.last_w is not None:
                deps.append(b.last_w)
            deps.extend(b.readers)
        return deps

    def _emit_waits(self, eng, deps):
        w = self.waited[eng]
        need = {}
        for (k, v) in deps:
            if eng == "pe" and k == ("e", "pe"):
                continue
            if w.get(k, 0) < v and need.get(k, 0) < v:
                need[k] = v
        for k, v in need.items():
            w[k] = v
            self.items[eng].append(("wait", self.sems[k], v))

    def op(self, eng, fn, reads=(), writes=()):
        deps = self._deps(reads, writes)
        for b in reads:
            if b.psum:
                deps.extend(ev for ev in b.readers if ev[0] != ("e", eng))
        self._emit_waits(eng, deps)
        self.count[eng] += 1
        ev = (("e", eng), self.count[eng])
        self.items[eng].append(("op", fn, self.esem[eng], 1))
        for b in writes:
            b.last_w = ev
            b.readers = []
        for b in reads:
            if b not in writes:
                if len(b.readers) > 64:
                    best = {}
                    for (k, v) in b.readers:
                        if best.get(k, 0) < v:
                            best[k] = v
                    b.readers = list(best.items())
                b.readers.append(ev)
        return ev

    def dma(self, eng, fn, dst, reads=(), inc=16):
        if dst.sem is None:
            dst.sem = self.ctx.enter_context(self.nc.semaphore("ds_" + dst.name))
            self.sems[("d", id(dst))] = dst.sem
            self.dmabufs.append(dst)
        self._emit_waits(eng, self._deps(reads, [dst]))
        dst.ndma += inc
        ev = (("d", id(dst)), dst.ndma)
        self.items[eng].append(("op", fn, dst.sem, inc))
        dst.last_w = ev
        dst.readers = []
        for b in reads:
            b.readers.append(ev)
        return ev

    def wait_all(self, eng, bufs):
        self._emit_waits(eng, [b.last_w for b in bufs if b.last_w is not None])

    def barrier(self):
        deps = [(("e", e), self.count[e]) for e in self.ENGS if self.count[e] > 0]
        deps += [(("d", id(b)), b.ndma) for b in self.dmabufs if b.ndma > 0]
        for e in self.ENGS:
            self._emit_waits(e, deps)

    def finish(self):
        with self.nc.Block() as block:
            def runner(name):
                def run(e):
                    for it in self.items[name]:
                        if it[0] == "wait":
                            e.wait_ge(it[1], it[2])
                        else:
                            it[1](e).then_inc(it[2], it[3])
                return run
            block.sync(runner("sp"))
            block.tensor(runner("pe"))
            block.scalar(runner("act"))
            block.vector(runner("dve"))
            block.gpsimd(runner("pool"))


class Arena:
    def __init__(self, nc, ctx, nbytes):
        self.t = ctx.enter_context(nc.sbuf_tensor("arena", [128, nbytes // 4], F32))
        self.off = 0
        self.cap = nbytes

    def alloc(self, shape, dt):
        n = 1
        for s in shape:
            n *= s
        size = n * (4 if dt in (F32, I32) else 2)
        size = (size + 31) // 32 * 32
        assert self.off + size <= self.cap, ("SBUF arena overflow", self.off, size, self.cap)
        ap = self.t[:, self.off // 4:(self.off + size) // 4]
        self.off += size
        if dt != F32:
            ap = ap.bitcast(dt)
        ap = ap[:, 0:n]
        if len(shape) == 2:
            ap = ap.rearrange("p (a b) -> p a b", a=shape[0])
        elif len(shape) == 3:
            ap = ap.rearrange("p (a b c) -> p a b c", a=shape[0], b=shape[1])
        return ap


def _cst_layout():
    off = {}
    cur = 0
    for name, n in [("ident", 128), ("triI", 128), ("triX", 128), ("maskUUI", 256), ("maskL", 128),
                    ("bones", 128), ("DT", 512), ("kdec", 512), ("qdecT", 512), ("fac2", 64),
                    ("iota", 256), ("tokid", 16), ("trash", 2), ("g64", 512), ("tri01", 128), ("ones", 128)]:
        off[name] = (cur, n)
        cur += n
    return off, cur


CST_OFF, NCST = _cst_layout()


def build_cst():
    c = np.zeros((128, NCST), np.float32)
    p = np.arange(128)
    same = (p[:, None] // 64) == (p[None, :] // 64)

    def put(name, arr):
        o, n = CST_OFF[name]
        c[:, o:o + n] = arr.reshape(128, n)
    put("ident", np.eye(128))
    put("triI", -EM05 * ((p[:, None] <= p[None, :]) & same))
    put("triX", -EM05 * ((p[:, None] < p[None, :]) & same))
    mU = ((p[:, None] < p[None, :]) & same).astype(np.float32)
    mUI = ((p[:, None] <= p[None, :]) & same).astype(np.float32)
    put("maskUUI", np.concatenate([mU, mUI], 1))
    put("maskL", mU.T.copy())
    put("bones", same.astype(np.float32))
    DT = np.zeros((128, 4, 128)); kdec = np.zeros((128, 4, 128)); qdecT = np.zeros((128, 4, 128))
    fac2 = np.zeros((128, 16, 4)); g64 = np.zeros((128, 4, 128))
    for h in range(4):
        g = GAM[h]
        DT[:, h, :] = (g ** np.abs(p[:, None] - p[None, :])) * same * 128 ** -0.5
        kdec[:, h, :] = (g ** (63 - p % 64))[:, None] * 128 ** -0.5
        qdecT[:, h, :] = (g ** (p % 64 + 1))[None, :]
        for i in range(16):
            fac2[:, i, h] = g ** (128 * i + p + 1.0)
        g64[:, h, :] = g ** 64
    put("DT", DT); put("kdec", kdec); put("qdecT", qdecT); put("fac2", fac2); put("g64", g64)
    put("iota", np.tile(np.arange(256)[None, :], (128, 1)))
    put("tokid", 128 * np.arange(16)[None, :] + p[:, None])
    put("trash", 2 * SEG + 128 * np.arange(2)[None, :] + p[:, None])
    put("tri01", (p[:, None] < p[None, :]).astype(np.float32))
    put("ones", np.ones((128, 128)))
    return c


ROW_OFF = {"g1": (0, 1024), "g2": (1024, 1024), "gf": (2048, 1024), "rgn": (3072, 512), "wgn": (3584, 512),
           "br": (4096, 36)}
NROW = 4096 + 36
FT_MU, FT_W0, FT_A0, FT_KK, FT_KA, FT_RK, NFT = 0, 14, 18, 22, 26, 30, 34
NPC = 40


def build_program(dbg=False, with_moe=True, ntile1=NT, do_ret=True, do_rwkv=True, do_p2=True, do_ag=True, do_prev=True, rwkv_stage=9):
    nc = bass.Bass("TRN2", target_bir_lowering=False)

    def din(name, shape, dt=F32):
        return nc.dram_tensor(name, list(shape), dt, kind="ExternalInput").ap()
    d_x = din("x_ext", [SEG + 1, D])
    d_win = din("w_in", [D, 3840])
    d_cst = din("cst", [128, NCST])
    d_pc = din("pc", [128, NPC])
    d_row = din("rowp", [1, NROW])
    d_ft = din("ftab", [128, NFT])
    d_lora = din("lora", [128, 512])
    d_gup = din("gup", [128, 512])
    d_wout = din("w_out", [D, D])
    d_wr = din("wr", [D, 36])
    if with_moe:
        d_wg = din("w_gate", [NEXP, D, 512])
        d_wu = din("w_up", [NEXP, D, 512])
        d_wd = din("w_down", [NEXP, 512, D])
    d_cos = din("cos4", [SEG, 256])
    d_sin = din("sin4", [SEG, 256])
    d_out = nc.dram_tensor("out", [SEG, D], F32, kind="ExternalOutput").ap()
    d_dbg = nc.dram_tensor("dbg", [SEG, D], F32, kind="ExternalOutput").ap() if dbg else None
    s_ret = nc.dram_tensor("s_ret", [SEG, 512], F32).ap()
    s_y0 = nc.dram_tensor("s_y0", [SEG, 512], F32).ap()
    s_qT = nc.dram_tensor("s_qT", [NT, 128, 512], BF16).ap()
    s_zT = nc.dram_tensor("s_zT", [NT, 128, 512], BF16).ap()
    s_gate = nc.dram_tensor("s_gate", [SEG, 512], BF16).ap()
    s_bon = nc.dram_tensor("s_bon", [SEG, 512], BF16).ap()
    s_g = nc.dram_tensor("s_g", [SEG, 512], BF16).ap()
    ag_in = nc.dram_tensor("ag_in", [128, 1024], F32)
    ag_out = nc.dram_tensor("ag_out", [NCORE * 128, 1024], F32)
    s_xn2 = nc.dram_tensor("s_xn2", [SEG, D], BF16).ap()
    s_yd = nc.dram_tensor("s_yd", [2 * SEG + CAP, D], F32).ap()

    with contextlib.ExitStack() as ctx:
        fw = FW(nc, ctx)
        ar = Arena(nc, ctx, 207 * 1024)
        psb = []
        for k in range(8):
            t = ctx.enter_context(nc.psum_tensor("ps%d" % k, [128, 512], F32))
            psb.append((t, fw.buf("ps%d" % k)))
        pstate = {"i": 0}

        def psum():
            k = pstate["i"] % 6
            pstate["i"] += 1
            return psb[k]

        def tt(eng, out, in0, in1, op, r, w):
            fw.op(eng, lambda e: e.tensor_tensor(out=out, in0=in0, in1=in1, op=op), r, w)

        def ts(eng, out, in0, s1, s2, op0, op1, r, w):
            fw.op(eng, lambda e: e.tensor_scalar(out=out, in0=in0, scalar1=s1, scalar2=s2, op0=op0, op1=op1), r, w)

        def ts1(eng, out, in0, s1, op0, r, w):
            fw.op(eng, lambda e: e.tensor_single_scalar(out=out, in_=in0, scalar=s1, op=op0), r, w)

        def stt(out, in0, scalar, in1, op0, op1, r, w):
            fw.op("dve", lambda e: e.scalar_tensor_tensor(out=out, in0=in0, scalar=scalar, in1=in1, op0=op0, op1=op1), r, w)

        def act(out, in_, func, r, w, bias=None, scale=None):
            kw = {}
            if bias is not None:
                kw["bias"] = bias
            if scale is not None:
                kw["scale"] = scale
            fw.op("act", lambda e: e.activation(out=out, in_=in_, func=func, **kw), r, w)

        def cp(eng, out, in_, r, w):
            if eng == "act":
                fw.op("act", lambda e: e.activation(out=out, in_=in_, func=AF.Copy), r, w)
            else:
                fw.op(eng, lambda e: e.tensor_copy(out=out, in_=in_), r, w)

        def mm(out, lhsT, rhs, start, stop, r, w):
            fw.op("pe", lambda e: e.matmul(out, lhsT=lhsT, rhs=rhs, start=start, stop=stop), r, w)

        def tr(out, in_, ident, r, w):
            fw.op("pe", lambda e: e.transpose(out=out, in_=in_, identity=ident), r, w)

        def rsum(out, in_, r, w):
            fw.op("dve", lambda e: e.reduce_sum(out=out, in_=in_, axis=AX.X), r, w)

        def dma(eng, out, in_, dst, r=()):
            fw.dma(eng, lambda e: e.dma_start(out=out, in_=in_), dst, r)

        cst = ar.alloc([NCST], F32); b_cst = fw.buf("cst")
        b_row = fw.buf("rowb")
        ROWS = {}
        ftab = ar.alloc([NFT], F32); b_ft = fw.buf("ftab")
        pct = ar.alloc([NPC], F32); b_pc = fw.buf("pc")
        identb = ar.alloc([128], BF16); b_idb = fw.buf("identb")
        dma("sp", cst, d_cst[:, :], b_cst)
        dma("sp", ftab, d_ft[:, :], b_ft)
        dma("sp", pct, d_pc[:, :], b_pc)

        def C(name, lo=0, hi=None):
            o, n = CST_OFF[name]
            return cst[:, o + lo:o + (n if hi is None else hi)]

        def load_rows(names):
            for nm in names:
                o, n = ROW_OFF[nm]
                ROWS[nm] = ar.alloc([n], F32)
                dma("sp", ROWS[nm], d_row[0:1, o:o + n].partition_broadcast(128), b_row)

        def ROW(name):
            return ROWS[name]
        identf = C("ident")
        cp("dve", identb, identf, [b_cst], [b_idb])
        mark_persist = ar.off
        load_rows(["g1"])

        w_in = ar.alloc([8, 3840], BF16); b_win = [fw.buf("win0"), fw.buf("win1")]
        src = d_win.rearrange("(k p) c -> p k c", p=128)
        fw.dma("pool", lambda e: e.dma_start(out=w_in[:, :, 0:1920], in_=src[:, :, 0:1920]), b_win[0])
        fw.dma("pool", lambda e: e.dma_start(out=w_in[:, :, 1920:3840], in_=src[:, :, 1920:3840]), b_win[1])
        lorab = ar.alloc([512], BF16); b_lora = fw.buf("lora")
        gupb = ar.alloc([512], BF16); b_gup = fw.buf("gup")
        fw.dma("pool", lambda e: e.dma_start(out=lorab, in_=d_lora[:, :]), b_lora)
        fw.dma("pool", lambda e: e.dma_start(out=gupb, in_=d_gup[:, :]), b_gup)

        xt = [ar.alloc([D], F32) for _ in range(2)]; b_xt = [fw.buf("xt0"), fw.buf("xt1")]
        x0 = xt[1]; b_x0 = b_xt[1]
        st1 = ar.alloc([8], F32); b_st1 = fw.buf("st1")
        xs = ar.alloc([D], BF16); b_xs = fw.buf("xs")
        xnT = [ar.alloc([8, 128], BF16) for _ in range(2)]; b_xnT = [fw.buf("xnT0"), fw.buf("xnT1")]
        cosb = [ar.alloc([256], F32) for _ in range(2)]; b_cos = [fw.buf("cos0"), fw.buf("cos1")]
        sinb = [ar.alloc([256], F32) for _ in range(2)]; b_sin = [fw.buf("sin0"), fw.buf("sin1")]
        rtq = ar.alloc([D], F32); rt1 = rtq[:, 0:512]; rt2 = rtq[:, 512:1024]; b_rt = fw.buf("rt")
        sq = rtq; b_sq = b_rt
        qr = ar.alloc([512], BF16); kr = ar.alloc([512], BF16); b_qr = fw.buf("qr"); b_kr = fw.buf("kr")
        kd = ar.alloc([512], BF16); b_kd = fw.buf("kd")
        vb = ar.alloc([512], BF16); b_vb = fw.buf("vb")
        gateb = ar.alloc([512], BF16); b_gate = fw.buf("gateb")
        qkT = ar.alloc([8, 128], BF16); b_qkT = fw.buf("qkT")
        qdT = ar.alloc([4, 128], BF16); b_qdT = fw.buf("qdT")
        sT = ar.alloc([4, 128], BF16); b_sT = fw.buf("sT")
        Sf = ar.alloc([4, 128], F32); b_Sf = fw.buf("Sf")
        Sb2 = [ar.alloc([4, 128], BF16) for _ in range(2)]; b_Sb2 = [fw.buf("Sb0"), fw.buf("Sb1")]
        zb = ar.alloc([512], BF16); b_zb = fw.buf("zb")
        retp = ar.alloc([512], F32); b_retp = fw.buf("retp")
        fb1_ = ar.alloc([14, 129], F32); fb = [fb1_, fb1_]; b_fb1_ = fw.buf("fb"); b_fb = [b_fb1_, b_fb1_]
        ff = ar.alloc([14, 128], F32); b_ff = fw.buf("ff")
        twb = ar.alloc([128], BF16); b_tw = fw.buf("tw")
        sgb = ar.alloc([128], BF16); b_sg = fw.buf("sg")
        sig = ar.alloc([4, 128], F32); b_sig = fw.buf("sig")
        av = ar.alloc([4, 128], F32); b_av = fw.buf("av")
        kk = ar.alloc([4, 128], F32); b_kk = fw.buf("kk")
        kk2 = ar.alloc([4, 128], F32); b_kk2 = fw.buf("kk2")
        rn = ar.alloc([4, 128], F32); b_rn = fw.buf("rn")
        k2 = ar.alloc([4, 128], F32); b_k2 = fw.buf("k2")
        bv = ar.alloc([4, 128], F32); b_bv = fw.buf("bv")
        sigtm = ar.alloc([512], F32); b_sigtm = fw.buf("sigtm")
        eW = ar.alloc([4, 128], F32); eWn = ar.alloc([4, 128], F32); eWx = ar.alloc([4, 128], F32)
        b_eW = fw.buf("eW"); b_eWn = fw.buf("eWn"); b_eWx = fw.buf("eWx")
        AR = ar.alloc([4, 2, 128], BF16); b_AR = fw.buf("AR")
        Bt = ar.alloc([4, 128], BF16); Kt = ar.alloc([4, 128], BF16); vTb = ar.alloc([4, 128], BF16)
        b_Bt = fw.buf("Bt"); b_Kt = fw.buf("Kt"); b_vTb = fw.buf("vTb")
        rkb = ar.alloc([4, 128], BF16); b_rkb = fw.buf("rkb")
        TM = ar.alloc([4, 512], BF16); b_TM = fw.buf("TM")
        hexp = ar.alloc([128], BF16); b_hexp = fw.buf("hexp")
        bonb = ar.alloc([512], BF16); b_bon = fw.buf("bonb")
        gb = ar.alloc([512], BF16); b_gb = fw.buf("gb")
        y0 = ar.alloc([512], F32); b_y0 = fw.buf("y0")
        zTb = ar.alloc([4, 128], BF16); b_zTb = fw.buf("zTb")
        Nbuf = [ar.alloc([8, 128], BF16) for _ in range(2)]; b_Nbuf = [fw.buf("Nb0"), fw.buf("Nb1")]
        Abuf = [ar.alloc([8, 128], BF16) for _ in range(2)]; b_Abuf = [fw.buf("Ab0"), fw.buf("Ab1")]
        Ybuf = [ar.alloc([8, 128], BF16) for _ in range(2)]; b_Ybuf = [fw.buf("Yb0"), fw.buf("Yb1")]
        ArbT8 = ar.alloc([8, 128], BF16); b_ArbT8 = fw.buf("ArbT8")
        AakT8 = ar.alloc([8, 128], BF16); b_AakT8 = fw.buf("AakT8")
        ArkT8 = ar.alloc([8, 128], BF16); b_ArkT8 = fw.buf("ArkT8")
        RpT8 = ar.alloc([8, 128], BF16); b_RpT8 = fw.buf("RpT8")
        stF = [ar.alloc([128], F32) for _ in range(4)]; stB = [ar.alloc([128], BF16) for _ in range(4)]
        b_st = [fw.buf("st%d" % j) for j in range(4)]
        Gp = [[ar.alloc([128], BF16) for _ in range(2)] for _ in range(4)]; b_Gp = [[fw.buf() for _ in range(2)] for _ in range(4)]
        Npp = [[ar.alloc([128], F32) for _ in range(2)] for _ in range(4)]; b_Npp = [[fw.buf() for _ in range(2)] for _ in range(4)]
        print("arena after pass1 alloc", ar.off)

        cp("dve", hexp, C("bones"), [b_cst], [b_hexp])
        fw.op("pool", lambda e: e.memset(Sf, 0.0), (), [b_Sf])
        fw.op("pool", lambda e: e.memset(zb, 0.0), (), [b_zb])
        for j in range(4):
            fw.op("pool", (lambda j: lambda e: e.memset(stF[j], 0.0))(j), (), [b_st[j]])
            for hh in range(2):
                cp("pool", stF[j][64 * hh:64 * hh + 64, 64:128], identf[64 * hh:64 * hh + 64, 64 * hh:64 * hh + 64], [b_cst], [b_st[j]])
            cp("dve", stB[j], stF[j], [b_st[j]], [b_st[j]])
            for c in range(2):
                fw.op("pool", (lambda j, c: lambda e: e.memset(Gp[j][c], 0.0))(j, c), (), [b_Gp[j][c]])
                fw.op("pool", (lambda j, c: lambda e: e.memset(Npp[j][c], 0.0))(j, c), (), [b_Npp[j][c]])

        def load_norm_T(i, src_rows, xt_ap, b_x, xnT_ap, b_xn, n=128):
            dma("sp", xt_ap[0:n, :], src_rows, b_x)
            act(sq[0:n, :], xt_ap[0:n, :], AF.Square, [b_x], [b_sq])
            rsum(st1[0:n, 0:1], sq[0:n, :], [b_sq], [b_st1])
            act(st1[0:n, 1:2], st1[0:n, 0:1], AF.Sqrt, [b_st1], [b_st1], bias=1e-6, scale=1.0 / D)
            fw.op("dve", lambda e: e.reciprocal(out=st1[0:n, 2:3], in_=st1[0:n, 1:2]), [b_st1], [b_st1])
            stt(xs[0:n, :], xt_ap[0:n, :], st1[0:n, 2:3], ROW("g1")[0:n, :], ALU.mult, ALU.mult, [b_x, b_st1, b_row], [b_xs])
            pt, bp = psum()
            ptb = pt[:].bitcast(BF16)
            for kc in range(8):
                tr(ptb[:, kc * 128:kc * 128 + n], xs[0:n, kc * 128:(kc + 1) * 128], identb[0:n, 0:n], [b_xs, b_idb], [bp])
            if n == 128:
                cp("act", xnT_ap, ptb[:, 0:1024].rearrange("p (a b) -> p a b", a=8), [bp], [b_xn])
            else:
                cp("act", xnT_ap[:, :, 0:n], ptb[:, 0:1024].rearrange("p (a b) -> p a b", a=8)[:, :, 0:n], [bp], [b_xn])

        if do_prev:
            load_norm_T(-1, d_x[0:1, :], x0, b_x0, xnT[1], b_xnT[1], n=1)
            for g4 in range(4):
                pt, bp = psum()
                nchunk = 4 if g4 < 3 else 2
                for cc in range(nchunk):
                    ch = g4 * 4 + cc
                    for kc in range(8):
                        mm(pt[:, cc:cc + 1], w_in[:, kc, 2048 + ch * 128:2048 + (ch + 1) * 128], xnT[1][:, kc, 0:1],
                           kc == 0, kc == 7, [b_win[0], b_win[1], b_xnT[1]], [bp])
                cp("act", fb[0][:, g4 * 4:g4 * 4 + nchunk, 0], pt[:, 0:nchunk], [bp], [b_fb[0]])

        b_sret = fw.buf("s_ret"); b_sy0 = fw.buf("s_y0"); b_sqT = fw.buf("s_qT"); b_szT = fw.buf("s_zT")
        b_sgate = fw.buf("s_gate"); b_sbon = fw.buf("s_bon"); b_sg = fw.buf("s_g")

        for i in range(ntile1):
            par = i % 2
            r0 = 1 + i * 128
            load_norm_T(i, d_x[r0:r0 + 128, :], xt[par], b_xt[par], xnT[par], b_xnT[par])
            dma("sp", cosb[par], d_cos[i * 128:(i + 1) * 128, :], b_cos[par])
            dma("sp", sinb[par], d_sin[i * 128:(i + 1) * 128, :], b_sin[par])
            XN = xnT[par]; bXN = b_xnT[par]
            wdeps = [b_win[0], b_win[1], bXN]
            if do_ret:
                pq, bq = psum(); pk, bk = psum(); pv, bvp = psum(); pg, bg = psum()
                for kc in range(8):
                    for (pp, bb, c0) in ((pq, bq, 0), (pk, bk, 512), (pv, bvp, 1024), (pg, bg, 1536)):
                        mm(pp[:, :], XN[:, kc, :], w_in[:, kc, c0:c0 + 512], kc == 0, kc == 7, wdeps, [bb])
                cs = cosb[par].rearrange("p (h x) -> p h x", h=4)[:, :, 0:64]
                sn = sinb[par].rearrange("p (h x) -> p h x", h=4)[:, :, 0:64]
                for (pp, bb, dst, bd) in ((pq, bq, qr, b_qr), (pk, bk, kr, b_kr)):
                    v4 = pp[:, :].rearrange("p (h t x) -> p h t x", h=4, t=2)
                    d4 = dst.rearrange("p (h t x) -> p h t x", h=4, t=2)
                    a4 = rt1.rearrange("p (h t x) -> p h t x", h=4, t=2)
                    c4 = rt2.rearrange("p (h t x) -> p h t x", h=4, t=2)
                    rdeps = [bb, b_cos[par], b_sin[par]]
                    tt("dve", a4[:, :, 0, :], v4[:, :, 0, :], cs, ALU.mult, rdeps, [b_rt])
                    tt("dve", a4[:, :, 1, :], v4[:, :, 1, :], sn, ALU.mult, rdeps, [b_rt])
                    tt("dve", c4[:, :, 0, :], v4[:, :, 0, :], sn, ALU.mult, rdeps, [b_rt])
                    tt("dve", c4[:, :, 1, :], v4[:, :, 1, :], cs, ALU.mult, rdeps, [b_rt])
                    tt("pool", d4[:, :, 0, :], a4[:, :, 0, :], a4[:, :, 1, :], ALU.subtract, [b_rt], [bd])
                    tt("pool", d4[:, :, 1, :], c4[:, :, 0, :], c4[:, :, 1, :], ALU.add, [b_rt], [bd])
                tt("pool", kd, kr, C("kdec"), ALU.mult, [b_kr, b_cst], [b_kd])
                cp("act", vb, pv[:, :], [bvp], [b_vb])
                act(gateb, pg[:, :], AF.Silu, [bg], [b_gate])
                pt, bp = psum(); ptb = pt[:].bitcast(BF16)
                for h in range(4):
                    tr(ptb[:, h * 128:(h + 1) * 128], qr[:, h * 128:(h + 1) * 128], identb, [b_qr, b_idb], [bp])
                    tr(ptb[:, (4 + h) * 128:(5 + h) * 128], kr[:, h * 128:(h + 1) * 128], identb, [b_kr, b_idb], [bp])
                cp("act", qkT, ptb[:, 0:1024].rearrange("p (a b) -> p a b", a=8), [bp], [b_qkT])
                tt("pool", qdT, qkT[:, 0:4, :], C("qdecT").rearrange("p (a b) -> p a b", a=4), ALU.mult, [b_qkT, b_cst], [b_qdT])
                ps_, bs_ = psum()
                for h in range(4):
                    mm(ps_[:, h * 128:(h + 1) * 128], qkT[:, 4 + h, :], qkT[:, h, :], True, True, [b_qkT], [bs_])
                tt("dve", sT, ps_[:, :].rearrange("p (a b) -> p a b", a=4), C("DT").rearrange("p (a b) -> p a b", a=4), ALU.mult, [bs_, b_cst], [b_sT])
                pkv = [psum(), psum()]
                for c in range(2):
                    for h in range(4):
                        mm(pkv[c][0][:, h * 128:(h + 1) * 128], kd[64 * c:64 * c + 64, h * 128:(h + 1) * 128],
                           vb[64 * c:64 * c + 64, h * 128:(h + 1) * 128], True, True, [b_kd, b_vb], [pkv[c][1]])
                cp("act", Sb2[0], Sf, [b_Sf], [b_Sb2[0]])
                for c in range(2):
                    tt("pool", Sf, Sf, C("g64").rearrange("p (a b) -> p a b", a=4), ALU.mult, [b_Sf, b_cst], [b_Sf])
                    tt("dve", Sf, Sf, pkv[c][0][:, :].rearrange("p (a b) -> p a b", a=4), ALU.add, [b_Sf, pkv[c][1]], [b_Sf])
                    if c == 0:
                        cp("act", Sb2[1], Sf, [b_Sf], [b_Sb2[1]])
                po, bo = psum()
                for h in range(4):
                    mm(po[:, h * 128:(h + 1) * 128], sT[:, h, :], vb[:, h * 128:(h + 1) * 128], True, False, [b_sT, b_vb], [bo])
                    for c in range(2):
                        mm(po[64 * c:64 * c + 64, h * 128:(h + 1) * 128], qdT[:, h, 64 * c:64 * c + 64], Sb2[c][:, h, :], False, (c == 1),
                           [b_qdT, b_Sb2[c]], [bo])
                cp("act", retp, po[:, :], [bo], [b_retp])
                dma("sp", s_ret[i * 128:(i + 1) * 128, :], retp, b_sret, [b_retp])
                dma("sp", s_qT[i, :, :], qkT[:, 0:4, :].rearrange("p a b -> p (a b)"), b_sqT, [b_qkT])
                dma("sp", s_gate[i * 128:(i + 1) * 128, :], gateb, b_sgate, [b_gate])

            for _once in ([0] if do_rwkv else []):
                FB = fb[par]; bFB = b_fb[par]
                for g4 in range(4):
                    pt, bp = psum()
                    nchunk = 4 if g4 < 3 else 2
                    for cc in range(nchunk):
                        ch = g4 * 4 + cc
                        for kc in range(8):
                            mm(pt[:, cc * 128:(cc + 1) * 128], w_in[:, kc, 2048 + ch * 128:2048 + (ch + 1) * 128], XN[:, kc, :],
                               kc == 0, kc == 7, wdeps, [bp])
                    cp("act", FB[:, g4 * 4:g4 * 4 + nchunk, 1:129], pt[:, 0:nchunk * 128].rearrange("p (a b) -> p a b", a=nchunk), [bp], [bFB])
                tt("pool", ff, FB[:, :, 0:128], FB[:, :, 1:129], ALU.subtract, [bFB], [b_ff])
                tt("pool", ff, ff, ftab[:, FT_MU:FT_MU + 14].unsqueeze(2).to_broadcast([128, 14, 128]), ALU.mult, [b_ff, b_ft], [b_ff])
                tt("dve", ff, ff, FB[:, :, 1:129], ALU.add, [b_ff, bFB], [b_ff])
                cp("pool", FB[:, :, 0:1], FB[:, :, 128:129], [bFB], [bFB])
                if rwkv_stage < 2: break
                act(twb[0:64, :], ff[0:64, 12, :], AF.Tanh, [b_ff], [b_tw])
                cp("pool", twb[64:128, :], ff[64:128, 12, :], [b_ff], [b_tw])
                act(sgb, ff[:, 13, :], AF.Sigmoid, [b_ff], [b_sg])
                pz, bz = psum(); pa, ba = psum(); pgg, bgg = psum()
                for j in range(4):
                    mm(pz[:, j * 128:(j + 1) * 128], lorab[0:64, j * 128:(j + 1) * 128], twb[0:64, :], True, True, [b_lora, b_tw], [bz])
                    mm(pa[:, j * 128:(j + 1) * 128], lorab[64:128, j * 128:(j + 1) * 128], twb[64:128, :], True, True, [b_lora, b_tw], [ba])
                mm(pgg[:, :], sgb, gupb, True, True, [b_sg, b_gup], [bgg])
                cp("act", gb, pgg[:, :], [bgg], [b_gb])

                def bc4(col):
                    return ftab[:, col:col + 4].unsqueeze(2).to_broadcast([128, 4, 128])
                v4 = lambda p: p[:, :].rearrange("p (a b) -> p a b", a=4)
                tt("dve", sig, v4(pz), bc4(FT_W0), ALU.add, [bz, b_ft], [b_sig])
                act(sig, sig, AF.Sigmoid, [b_sig], [b_sig])
                tt("dve", av, v4(pa), bc4(FT_A0), ALU.add, [ba, b_ft], [b_av])
                act(av, av, AF.Sigmoid, [b_av], [b_av])
                tt("pool", kk, ff[:, 4:8, :], bc4(FT_KK), ALU.mult, [b_ff, b_ft], [b_kk])
                tt("pool", kk2, kk, kk, ALU.mult, [b_kk], [b_kk2])
                pss, bss = psum()
                mm(pss[:, :], C("bones"), kk2.rearrange("p a b -> p (a b)"), True, True, [b_cst, b_kk2], [bss])
                act(rn, v4(pss), AF.Sqrt, [bss], [b_rn])
                ts1("dve", rn, rn, 1e-12, ALU.max, [b_rn], [b_rn])
                fw.op("dve", lambda e: e.reciprocal(out=rn, in_=rn), [b_rn], [b_rn])
                tt("dve", kk, kk, rn, ALU.mult, [b_kk, b_rn], [b_kk])
                ts1("pool", k2, av, -1.0, ALU.add, [b_av], [b_k2])
                tt("pool", k2, k2, bc4(FT_KA), ALU.mult, [b_k2, b_ft], [b_k2])
                ts1("pool", k2, k2, 1.0, ALU.add, [b_k2], [b_k2])
                tt("pool", k2, k2, ff[:, 4:8, :], ALU.mult, [b_k2, b_ff], [b_k2])
                tt("dve", bv, kk, av, ALU.mult, [b_kk, b_av], [b_bv])
                if rwkv_stage < 3: break
                ptt, bptt = psum()
                for j in range(4):
                    tr(ptt[:, j * 128:(j + 1) * 128], sig[:, j, :], identf, [b_sig, b_cst], [bptt])
                cp("act", sigtm, ptt[:, :], [bptt], [b_sigtm])
                pwi, bwi = psum(); pwx, bwx = psum()
                for j in range(4):
                    mm(pwi[:, j * 128:(j + 1) * 128], sigtm[:, j * 128:(j + 1) * 128], C("triI"), True, True, [b_sigtm, b_cst], [bwi])
                    mm(pwx[:, j * 128:(j + 1) * 128], sigtm[:, j * 128:(j + 1) * 128], C("triX"), True, True, [b_sigtm, b_cst], [bwx])
                act(eW, v4(pwi), AF.Exp, [bwi], [b_eW])
                act(eWn, v4(pwi), AF.Exp, [bwi], [b_eWn], scale=-1.0)
                act(eWx, v4(pwx), AF.Exp, [bwx], [b_eWx])
                stt(AR[:, :, 0, :], kk, -1.0, eWx, ALU.mult, ALU.mult, [b_kk, b_eWx], [b_AR])
                tt("dve", AR[:, :, 1, :], ff[:, 0:4, :], eW, ALU.mult, [b_ff, b_eW], [b_AR])
                tt("dve", Bt, bv, eWn, ALU.mult, [b_bv, b_eWn], [b_Bt])
                tt("dve", Kt, k2, eWn, ALU.mult, [b_k2, b_eWn], [b_Kt])
                cp("pool", vTb, ff[:, 8:12, :], [b_ff], [b_vTb])
                tt("pool", rn, ff[:, 0:4, :], k2, ALU.mult, [b_ff, b_k2], [b_rn])
                tt("pool", rkb, rn, bc4(FT_RK), ALU.mult, [b_rn, b_ft], [b_rkb])
                if rwkv_stage < 4: break
                for (src4, bsrc, slot) in ((None, b_AR, 0), (Bt, b_Bt, 1), (Kt, b_Kt, 2), (vTb, b_vTb, 3)):
                    pt, bp = psum(); ptb = pt[:].bitcast(BF16)
                    for j in range(4):
                        s_ap = AR[:, j, 0, :] if src4 is None else src4[:, j, :]
                        tr(ptb[:, j * 128:(j + 1) * 128], s_ap, identb, [bsrc, b_idb], [bp])
                    cp("act" if slot % 2 == 0 else "dve", TM[:, slot, :], ptb[:, 0:512], [bp], [b_TM])
                Atm = TM[:, 0, :]; Btm = TM[:, 1, :]; Ktm = TM[:, 2, :]; Vtm = TM[:, 3, :]
                pbn, bbn = psum()
                for j in range(4):
                    mm(pbn[:, j * 128:(j + 1) * 128], rkb[:, j, :], hexp, True, True, [b_rkb, b_hexp], [bbn])
                tt("dve", bonb, pbn[:, :], Vtm, ALU.mult, [bbn, b_TM], [b_bon])

                if rwkv_stage < 5: break
                pYr = [psb[6], psb[7]]
                for rt_ in range(2):
                    mm(pYr[rt_][0][:, :], zb[:, 0:128], zb[:, 0:512], True, False, [b_zb], [pYr[rt_][1]])
                mU = C("maskUUI", 0, 128); mUI = C("maskUUI", 128, 256)
                for h in range(8):
                    j = h // 2; hh = h % 2
                    rows = slice(64 * hh, 64 * hh + 64)
                    pm1, bm1 = psum(); pm2, bm2 = psum()
                    ARh = AR[rows, j, :, :].rearrange("p a b -> p (a b)")
                    mm(pm1[:, 0:256], Bt[rows, j, :], ARh, True, True, [b_Bt, b_AR], [bm1])
                    mm(pm2[:, 0:256], Kt[rows, j, :], ARh, True, True, [b_Kt, b_AR], [bm2])
                    mm(pm1[:, 256:384], AR[rows, j, 0, :], Bt[rows, j, :], True, True, [b_AR, b_Bt], [bm1])
                    tt("dve", Nbuf[0][:, h, :], pm1[:, 0:128], mU, ALU.mult, [bm1, b_cst], [b_Nbuf[0]])
                    tt("dve", ArbT8[:, h, :], pm1[:, 128:256], mUI, ALU.mult, [bm1, b_cst], [b_ArbT8])
                    tt("dve", Abuf[0][:, h, :], pm1[:, 256:384], C("maskL"), ALU.mult, [bm1, b_cst], [b_Abuf[0]])
                    tt("dve", AakT8[:, h, :], pm2[:, 0:128], mU, ALU.mult, [bm2, b_cst], [b_AakT8])
                    tt("dve", ArkT8[:, h, :], pm2[:, 128:256], mUI, ALU.mult, [bm2, b_cst], [b_ArkT8])
                if rwkv_stage < 6: break
                px, bx = psum()
                for h in range(8):
                    mm(px[:, 64 * h:64 * h + 64], AakT8[:, h, :], Vtm[:, 64 * h:64 * h + 64], True, True, [b_AakT8, b_TM], [bx])
                cp("pool", Ybuf[0][:, :, 0:64], Atm.rearrange("p (a b) -> p a b", a=8), [b_TM], [b_Ybuf[0]])
                cp("act", Ybuf[0][:, :, 64:128], px[:, :].rearrange("p (a b) -> p a b", a=8), [bx], [b_Ybuf[0]])
                for l in range(6):
                    cur = l % 2; nxt = 1 - cur
                    for g in range(2):
                        pyl, byl = psum()
                        for hl in range(4):
                            h = 4 * g + hl
                            mm(pyl[:, hl * 128:(hl + 1) * 128], Nbuf[cur][:, h, :], Ybuf[cur][:, h, :], True, True,
                               [b_Nbuf[cur], b_Ybuf[cur]], [byl])
                        if l < 5:
                            psn, bsn = psum()
                            for hl in range(4):
                                h = 4 * g + hl
                                mm(psn[:, hl * 128:(hl + 1) * 128], Abuf[cur][:, h, :], Nbuf[cur][:, h, :], True, True,
                                   [b_Abuf[cur], b_Nbuf[cur]], [bsn])
                            cp("act", Nbuf[nxt][:, 4 * g:4 * g + 4, :], psn[:, :].rearrange("p (a b) -> p a b", a=4), [bsn], [b_Nbuf[nxt]])
                        if l < 4:
                            psa, bsa = psum()
                            for hl in range(4):
                                h = 4 * g + hl
                                mm(psa[:, hl * 128:(hl + 1) * 128], Nbuf[cur][:, h, :], Abuf[cur][:, h, :], True, True,
                                   [b_Abuf[cur], b_Nbuf[cur]], [bsa])
                            cp("act", Abuf[nxt][:, 4 * g:4 * g + 4, :], psa[:, :].rearrange("p (a b) -> p a b", a=4), [bsa], [b_Abuf[nxt]])
                        tt("dve", Ybuf[nxt][:, 4 * g:4 * g + 4, :], pyl[:, :].rearrange("p (a b) -> p a b", a=4), Ybuf[cur][:, 4 * g:4 * g + 4, :],
                           ALU.add, [byl, b_Ybuf[cur]], [b_Ybuf[nxt]])
                if rwkv_stage < 7: break
                PQ8 = Ybuf[0]; bPQ = b_Ybuf[0]
                pgc = [[psum(), psum()] for _ in range(2)]
                for c in range(2):
                    tr_ = slice(64 * c, 64 * c + 64)
                    for h in range(8):
                        j = h // 2; hh = h % 2
                        rows = slice(64 * hh, 64 * hh + 64); hc = slice(64 * h, 64 * h + 64)
                        pg_, bg_ = pgc[c][j // 2]
                        o = (j % 2) * 192
                        mm(pg_[rows, o:o + 64], PQ8[tr_, h, 0:64], Btm[tr_, hc], True, True, [bPQ, b_TM], [bg_])
                        mm(pg_[rows, o + 64:o + 128], Btm[tr_, hc], PQ8[tr_, h, 64:128], True, False, [bPQ, b_TM], [bg_])
                        mm(pg_[rows, o + 64:o + 128], Ktm[tr_, hc], Vtm[tr_, hc], False, True, [b_TM], [bg_])
                        mm(pg_[rows, o + 128:o + 192], PQ8[tr_, h, 0:64], ArbT8[tr_, h, tr_], True, True, [bPQ, b_ArbT8], [bg_])
                for h in range(8):
                    j = h // 2; hh = h % 2
                    rows = slice(64 * hh, 64 * hh + 64)
                    for c in range(2):
                        pg_, bg_ = pgc[c][j // 2]
                        o = (j % 2) * 192
                        tt("dve", Gp[j][c][rows, rows], pg_[rows, o:o + 64], identf[rows, rows], ALU.add, [bg_, b_cst], [b_Gp[j][c]])
                        ts1("dve", Npp[j][c][rows, 0:64], pg_[rows, o + 64:o + 128], eW[rows, j, 64 * c + 63:64 * c + 64], ALU.mult,
                            [bg_, b_eW], [b_Npp[j][c]])
                        tt("dve", RpT8[rows, h, 64 * c:64 * c + 64], pg_[rows, o + 128:o + 192], AR[rows, j, 1, 64 * c:64 * c + 64], ALU.add,
                           [bg_, b_AR], [b_RpT8])
                for c in range(2):
                    tr_ = slice(64 * c, 64 * c + 64)
                    for h in range(8):
                        hc = slice(64 * h, 64 * h + 64)
                        mm(pYr[c][0][tr_, hc], ArbT8[tr_, h, tr_], PQ8[tr_, h, 64:128], False, False, [b_ArbT8, bPQ], [pYr[c][1]])
                        mm(pYr[c][0][tr_, hc], ArkT8[tr_, h, tr_], Vtm[tr_, hc], False, False, [b_ArkT8, b_TM], [pYr[c][1]])
                if rwkv_stage < 8: break
                for j in range(4):
                    pzh = [psum(), psum()]
                    for c in range(2):
                        tr_ = slice(64 * c, 64 * c + 64)
                        for h2 in (2 * j, 2 * j + 1):
                            q2 = h2 % 2; r2 = slice(64 * q2, 64 * q2 + 64); hc2 = slice(64 * h2, 64 * h2 + 64)
                            mm(pYr[q2][0][tr_, hc2], RpT8[r2, h2, tr_], stB[j][r2, 0:64], False, True, [b_RpT8, b_st[j]], [pYr[q2][1]])
                            mm(pzh[q2][0][r2, 64 * c:64 * c + 64], stB[j][r2, 64:128], RpT8[r2, h2, tr_], True, True,
                               [b_RpT8, b_st[j]], [pzh[q2][1]])
                        pc_, bc_ = psum()
                        mm(pc_[:, 0:128], Gp[j][c], stB[j], True, True, [b_Gp[j][c], b_st[j]], [bc_])
                        stt(stF[j], pc_[:, 0:128], eW[:, j, 64 * c + 63:64 * c + 64], Npp[j][c], ALU.mult, ALU.add,
                            [bc_, b_eW, b_Npp[j][c]], [b_st[j]])
                        cp("act", stB[j], stF[j], [b_st[j]], [b_st[j]])
                    for q2 in range(2):
                        r2 = slice(64 * q2, 64 * q2 + 64)
                        cp("act", zTb[r2, j, :], pzh[q2][0][r2, 0:128], [pzh[q2][1]], [b_zTb])
                cp("act", y0, pYr[0][0][:, :], [pYr[0][1]], [b_y0])
                tt("dve", y0, y0, pYr[1][0][:, :], ALU.add, [b_y0, pYr[1][1]], [b_y0])
                dma("sp", s_y0[i * 128:(i + 1) * 128, :], y0, b_sy0, [b_y0])
                dma("sp", s_zT[i, :, :], zTb.rearrange("p a b -> p (a b)"), b_szT, [b_zTb])
                dma("sp", s_bon[i * 128:(i + 1) * 128, :], bonb, b_sbon, [b_bon])
                dma("sp", s_g[i * 128:(i + 1) * 128, :], gb, b_sg, [b_gb])

        agsb = ar.alloc([1024], F32); b_agsb = fw.buf("agsb")
        cp("act", agsb[:, 0:512], Sf.rearrange("p a b -> p (a b)"), [b_Sf], [b_agsb])
        for j in range(4):
            cp("dve", agsb[:, 512 + j * 64:512 + j * 64 + 64], stF[j][:, 0:64], [b_st[j]], [b_agsb])
        cp("pool", agsb[:, 768:1024], agsb[:, 512:768], [b_agsb], [b_agsb])
        b_agin = fw.buf("agin"); b_agout = fw.buf("agout")
        if do_ag:
            fw.dma("pool", lambda e: e.dma_start(out=ag_in.ap(), in_=agsb), b_agin, [b_agsb])
            fw.wait_all("pool", [b_agin])
            fw.dma("pool", lambda e: e.collective_compute("AllGather", ALU.bypass, replica_groups=[list(range(NCORE))],
                                                          ins=[ag_in.ap().opt()], outs=[ag_out.ap().opt()]),
                   b_agout, [b_agin], inc=1)
        fw.barrier()
        ar.off = mark_persist
        h1 = ar.alloc([NT, D], F32); b_h1 = [fw.buf("h1_%d" % i) for i in range(NT)]
        mark_h1 = ar.off
        agall = ar.alloc([NCORE, 1024], F32); b_agall = fw.buf("agall")
        dma("sp", agall, ag_out.ap().rearrange("(r p) f -> p r f", p=128), b_agall, [b_agout])
        load_rows(["rgn", "wgn"])
        w_out = ar.alloc([8, D], BF16); b_wout = fw.buf("wout")
        fw.dma("pool", lambda e: e.dma_start(out=w_out, in_=d_wout.rearrange("(k p) c -> p k c", p=128)), b_wout)
        SinF = ar.alloc([4, 128], F32); b_SinF = fw.buf("SinF")
        SinB = ar.alloc([4, 128], BF16); b_SinB = fw.buf("SinB")
        RinF = ar.alloc([4, 64], F32); b_RinF = fw.buf("RinF")
        RinB = ar.alloc([4, 64], BF16); b_RinB = fw.buf("RinB")
        for h in range(4):
            for r in range(NCORE):
                src_ = agall[:, r, h * 128:(h + 1) * 128]
                cf = pct[:, r * 4 + h:r * 4 + h + 1]
                if r == 0:
                    ts1("dve", SinF[:, h, :], src_, cf, ALU.mult, [b_agall, b_pc], [b_SinF])
                else:
                    stt(SinF[:, h, :], src_, cf, SinF[:, h, :], ALU.mult, ALU.add, [b_agall, b_pc, b_SinF], [b_SinF])
        cp("act", SinB, SinF, [b_SinF], [b_SinB])
        for r in range(NCORE):
            src_ = agall[:, r, 512:768].rearrange("p (a b) -> p a b", a=4)
            cf = pct[:, 32 + r:33 + r]
            if r == 0:
                ts1("dve", RinF, src_, cf, ALU.mult, [b_agall, b_pc], [b_RinF])
            else:
                stt(RinF.rearrange("p a b -> p (a b)"), agall[:, r, 512:768], cf, RinF.rearrange("p a b -> p (a b)"), ALU.mult, ALU.add,
                    [b_agall, b_pc, b_RinF], [b_RinF])
        cp("act", RinB, RinF, [b_RinF], [b_RinB])

        L = {}
        for nm, shp, dt_ in (("ret", [512], F32), ("y0", [512], F32), ("qT", [4, 128], BF16), ("zT", [4, 128], BF16),
                             ("gate", [512], BF16), ("bon", [512], BF16), ("g", [512], BF16), ("x", [D], F32)):
            L[nm] = [ar.alloc(shp, dt_) for _ in range(2)]
            L["b_" + nm] = [fw.buf("L%s0" % nm), fw.buf("L%s1" % nm)]
        hn = ar.alloc([512], F32); b_hn = fw.buf("hn")
        hsq = ar.alloc([512], F32); b_hsq = fw.buf("hsq")
        hst = ar.alloc([32], F32); b_hst = fw.buf("hst")
        mixb = ar.alloc([D], BF16); b_mix = fw.buf("mixb")
        mixT = ar.alloc([8, 128], BF16); b_mixT = fw.buf("mixT")
        print("arena after pass2 alloc", ar.off)

        def headnorm(src, bsrc, nh, hd, eps, gain_row, dst_f32):
            s3 = src.rearrange("p (a b) -> p a b", a=nh)
            rsum(hst[:, 0:nh], s3, [bsrc], [b_hst])
            ts1("dve", hst[:, 0:nh], hst[:, 0:nh], -1.0 / hd, ALU.mult, [b_hst], [b_hst])
            d3 = dst_f32.rearrange("p (a b) -> p a b", a=nh)
            tt("dve", d3, s3, hst[:, 0:nh].unsqueeze(2).to_broadcast([128, nh, hd]), ALU.add, [bsrc, b_hst], [b_hn])
            tt("pool", hsq.rearrange("p (a b) -> p a b", a=nh), d3, d3, ALU.mult, [b_hn], [b_hsq])
            rsum(hst[:, 8:8 + nh], hsq.rearrange("p (a b) -> p a b", a=nh), [b_hsq], [b_hst])
            act(hst[:, 16:16 + nh], hst[:, 8:8 + nh], AF.Sqrt, [b_hst], [b_hst], bias=eps, scale=1.0 / hd)
            fw.op("dve", lambda e: e.reciprocal(out=hst[:, 24:24 + nh], in_=hst[:, 16:16 + nh]), [b_hst], [b_hst])
            tt("dve", d3, d3, hst[:, 24:24 + nh].unsqueeze(2).to_broadcast([128, nh, hd]), ALU.mult, [b_hn, b_hst], [b_hn])
            tt("dve", dst_f32, dst_f32, gain_row, ALU.mult, [b_hn, b_row], [b_hn])

        for i in range(NT if do_p2 else 0):
            par = i % 2
            rs = slice(i * 128, (i + 1) * 128)
            dma("sp", L["ret"][par], s_ret[rs, :], L["b_ret"][par], [b_sret])
            dma("sp", L["y0"][par], s_y0[rs, :], L["b_y0"][par], [b_sy0])
            dma("sp", L["qT"][par].rearrange("p a b -> p (a b)"), s_qT[i, :, :], L["b_qT"][par], [b_sqT])
            dma("sp", L["zT"][par].rearrange("p a b -> p (a b)"), s_zT[i, :, :], L["b_zT"][par], [b_szT])
            dma("sp", L["gate"][par], s_gate[rs, :], L["b_gate"][par], [b_sgate])
            dma("sp", L["bon"][par], s_bon[rs, :], L["b_bon"][par], [b_sbon])
            dma("sp", L["g"][par], s_g[rs, :], L["b_g"][par], [b_sg])
            dma("sp", L["x"][par], d_x[1 + i * 128:1 + (i + 1) * 128, :], L["b_x"][par])
            pr_, br_ = psum()
            for h in range(4):
                mm(pr_[:, h * 128:(h + 1) * 128], L["qT"][par][:, h, :], SinB[:, h, :], True, True, [L["b_qT"][par], b_SinB], [br_])
            fo = CST_OFF["fac2"][0]
            for h in range(4):
                stt(L["ret"][par][:, h * 128:(h + 1) * 128], pr_[:, h * 128:(h + 1) * 128], cst[:, fo + i * 4 + h:fo + i * 4 + h + 1],
                    L["ret"][par][:, h * 128:(h + 1) * 128], ALU.mult, ALU.add, [br_, b_cst, L["b_ret"][par]], [L["b_ret"][par]])
            headnorm(L["ret"][par], L["b_ret"][par], 4, 128, 1e-5, ROW("rgn"), hn)
            tt("dve", mixb[:, 0:512], hn, L["gate"][par], ALU.mult, [b_hn, L["b_gate"][par]], [b_mix])
            py2 = [psum(), psum()]
            for h in range(8):
                j = h // 2; q2 = h % 2; r2 = slice(64 * q2, 64 * q2 + 64)
                mm(py2[q2][0][:, 64 * h:64 * h + 64], L["zT"][par][r2, j, :], RinB[r2, j, :], True, True, [L["b_zT"][par], b_RinB], [py2[q2][1]])
            for q2 in range(2):
                yv = L["y0"][par].rearrange("p (j t v) -> p j t v", j=4, t=2)[:, :, q2, :]
                pv_ = py2[q2][0][:, :].rearrange("p (j t v) -> p j t v", j=4, t=2)[:, :, q2, :]
                tt("dve", yv, yv, pv_, ALU.add, [L["b_y0"][par], py2[q2][1]], [L["b_y0"][par]])
            headnorm(L["y0"][par], L["b_y0"][par], 8, 64, 64e-5, ROW("wgn"), hn)
            tt("dve", hn, hn, L["bon"][par], ALU.add, [b_hn, L["b_bon"][par]], [b_hn])
            tt("dve", mixb[:, 512:1024], hn, L["g"][par], ALU.mult, [b_hn, L["b_g"][par]], [b_mix])
            pt, bp = psum(); ptb = pt[:].bitcast(BF16)
            for kc in range(8):
                tr(ptb[:, kc * 128:(kc + 1) * 128], mixb[:, kc * 128:(kc + 1) * 128], identb, [b_mix, b_idb], [bp])
            cp("act", mixT, ptb[:, 0:1024].rearrange("p (a b) -> p a b", a=8), [bp], [b_mixT])
            for half in range(2):
                ph, bh = psum()
                for kc in range(8):
                    mm(ph[:, :], mixT[:, kc, :], w_out[:, kc, half * 512:(half + 1) * 512], kc == 0, kc == 7, [b_mixT, b_wout], [bh])
                tt("dve", h1[:, i, half * 512:(half + 1) * 512], ph[:, :], L["x"][par][:, half * 512:(half + 1) * 512], ALU.add,
                   [bh, L["b_x"][par]], [b_h1[i]])

        fw.barrier()
        ar.off = mark_h1
        b_out = fw.buf("out")
        load_rows(["g2", "gf", "br"])
        b_dbg = fw.buf("dbgout")
        if dbg:
            for i in range(NT):
                dma("sp", d_dbg[i * 128:(i + 1) * 128, :], h1[:, i, :], b_dbg, [b_h1[i]])
        if with_moe:
            mark_moe = ar.off
            BIG = 1.0e30
            wr_sb = ar.alloc([8, 36], F32); b_wr = fw.buf("wr")
            dma("sp", wr_sb, d_wr.rearrange("(k p) c -> p k c", p=128), b_wr)
            xn2f = ar.alloc([D], F32); b_xn2f = fw.buf("xn2f")
            xn2b = ar.alloc([D], BF16); b_xn2b = fw.buf("xn2b")
            xn2T = ar.alloc([8, 128], F32); b_xn2T = fw.buf("xn2T")
            RB = ar.alloc([NT, NEXP, 6], BF16); b_RALL = fw.buf("RB")
            wtmp = ar.alloc([3, NEXP], F32); whb = ar.alloc([NEXP], BF16); b_wtmp = fw.buf("wtmp")
            MF = ar.alloc([NT, NEXP], F32); b_MF = fw.buf("MF")
            MB = ar.alloc([NT, NEXP], BF16); b_MB = fw.buf("MB")
            POS = ar.alloc([NT, NEXP], F32); b_POS = fw.buf("POS")
            lg = ar.alloc([36], F32); b_lg = fw.buf("lg")
            rt = ar.alloc([16], F32); b_rtt = fw.buf("rtt")
            goh = ar.alloc([4], F32); pen = ar.alloc([4], F32); b_goh = fw.buf("goh")
            msk = ar.alloc([32], F32); msk2 = ar.alloc([32], F32); oh1 = ar.alloc([32], F32); oh2 = ar.alloc([32], F32)
            b_msk = fw.buf("msk"); b_oh = fw.buf("oh")
            tri01b = ar.alloc([128], BF16); onesb = ar.alloc([128], BF16); b_trib = fw.buf("trib")
            cp("dve", tri01b, C("tri01"), [b_cst], [b_trib])
            cp("dve", onesb, C("ones"), [b_cst], [b_trib])
            Sel = ar.alloc([NT, CAP], BF16); b_Sel = fw.buf("Sel")
            idxf = ar.alloc([2, 6], F32); b_idxf = fw.buf("idxf")
            wsl = ar.alloc([2], F32); gf_ = ar.alloc([2], F32)
            gidx = ar.alloc([2], I32); dsti = ar.alloc([2], I32); dstf = ar.alloc([2], F32); b_gidx = fw.buf("gidx"); b_dsti = fw.buf("dsti")
            xg = ar.alloc([2, D], BF16); b_xg = [fw.buf("xg0"), fw.buf("xg1")]
            xgT = ar.alloc([8, CAP], BF16); b_xgT = fw.buf("xgT")
            Wg = [ar.alloc([8, 512], BF16) for _ in range(2)]; Wu = [ar.alloc([8, 512], BF16) for _ in range(2)]
            Wd = [ar.alloc([4, D], BF16) for _ in range(2)]
            b_Wg = [fw.buf("Wg0"), fw.buf("Wg1")]; b_Wu = [fw.buf("Wu0"), fw.buf("Wu1")]; b_Wd = [fw.buf("Wd0"), fw.buf("Wd1")]
            hidT = ar.alloc([4, CAP], BF16); b_hidT = fw.buf("hidT")
            sgt = ar.alloc([CAP], F32); b_sgt = fw.buf("sgt")
            yout = ar.alloc([2, D], F32); b_yout = fw.buf("yout")
            b_sxn2 = fw.buf("s_xn2"); b_yd = fw.buf("s_yd")
            print("arena after moe alloc", ar.off)

            def load_w(e, p):
                fw.dma("pool", lambda en: en.dma_start(out=Wg[p], in_=d_wg[e].rearrange("(k p) c -> p k c", p=128)), b_Wg[p])
                fw.dma("pool", lambda en: en.dma_start(out=Wu[p], in_=d_wu[e].rearrange("(k p) c -> p k c", p=128)), b_Wu[p])
                fw.dma("pool", lambda en: en.dma_start(out=Wd[p], in_=d_wd[e].rearrange("(k p) c -> p k c", p=128)), b_Wd[p])
            load_w(0, 0)
            fw.op("pool", lambda e: e.memset(yout, 0.0), (), [b_yout])
            nrow_yd = 2 * SEG + CAP
            for r in range(nrow_yd // 256):
                dma("sp", s_yd[r * 256:(r + 1) * 256, :].rearrange("(p a) d -> p a d", a=2), yout, b_yd, [b_yout])
            cp("pool", RB[:, :, :, 0], C("tokid", 0, 1).unsqueeze(2).to_broadcast([128, NT, NEXP]), [b_cst], [b_RALL])
            for i in range(NT):
                act(xn2f, h1[:, i, :], AF.Square, [b_h1[i]], [b_xn2f])
                rsum(rt[:, 0:1], xn2f, [b_xn2f], [b_rtt])
                act(rt[:, 1:2], rt[:, 0:1], AF.Sqrt, [b_rtt], [b_rtt], bias=1e-6, scale=1.0 / D)
                fw.op("dve", lambda e: e.reciprocal(out=rt[:, 2:3], in_=rt[:, 1:2]), [b_rtt], [b_rtt])
                stt(xn2f, h1[:, i, :], rt[:, 2:3], ROW("g2"), ALU.mult, ALU.mult, [b_h1[i], b_rtt, b_row], [b_xn2f])
                cp("act", xn2b, xn2f, [b_xn2f], [b_xn2b])
                dma("sp", s_xn2[i * 128:(i + 1) * 128, :], xn2b, b_sxn2, [b_xn2b])
                for hf in range(2):
                    pt, bp = psum()
                    for q in range(4):
                        kc = hf * 4 + q
                        tr(pt[:, q * 128:(q + 1) * 128], xn2f[:, kc * 128:(kc + 1) * 128], identf, [b_xn2f, b_cst], [bp])
                    cp("act", xn2T[:, hf * 4:hf * 4 + 4, :], pt[:, :].rearrange("p (a b) -> p a b", a=4), [bp], [b_xn2T])
                pl, bl = psum()
                for kc in range(8):
                    mm(pl[:, 0:36], xn2T[:, kc, :], wr_sb[:, kc, :], kc == 0, kc == 7, [b_xn2T, b_wr], [bl])
                tt("dve", lg, pl[:, 0:36], ROW("br"), ALU.add, [bl, b_row], [b_lg])
                fw.op("dve", lambda e: e.reduce_max(out=rt[:, 3:4], in_=lg[:, 0:4], axis=AX.X), [b_lg], [b_rtt])
                ts1("dve", goh, lg[:, 0:4], rt[:, 3:4], ALU.is_equal, [b_lg, b_rtt], [b_goh])
                ts1("dve", rt[:, 4:5], rt[:, 3:4], -1.0, ALU.mult, [b_rtt], [b_rtt])
                act(pen, lg[:, 0:4], AF.Exp, [b_lg, b_rtt], [b_goh], bias=rt[:, 4:5])
                rsum(rt[:, 5:6], pen, [b_goh], [b_rtt])
                fw.op("dve", lambda e: e.reciprocal(out=rt[:, 6:7], in_=rt[:, 5:6]), [b_rtt], [b_rtt])
                ts("dve", pen, goh, -1.0, BIG, ALU.add, ALU.mult, [b_goh], [b_goh])
                tt("dve", msk.rearrange("p (a b) -> p a b", a=4), lg[:, 4:36].rearrange("p (a b) -> p a b", a=4),
                   pen.unsqueeze(2).to_broadcast([128, 4, 8]), ALU.add, [b_lg, b_goh], [b_msk])
                fw.op("dve", lambda e: e.reduce_max(out=rt[:, 7:8], in_=msk, axis=AX.X), [b_msk], [b_rtt])
                ts1("dve", oh1, msk, rt[:, 7:8], ALU.is_equal, [b_msk, b_rtt], [b_oh])
                stt(msk2, oh1, -BIG, msk, ALU.mult, ALU.add, [b_oh, b_msk], [b_msk])
                fw.op("dve", lambda e: e.reduce_max(out=rt[:, 8:9], in_=msk2, axis=AX.X), [b_msk], [b_rtt])
                ts1("dve", oh2, msk2, rt[:, 8:9], ALU.is_equal, [b_msk, b_rtt], [b_oh])
                tt("dve", rt[:, 9:10], rt[:, 8:9], rt[:, 7:8], ALU.subtract, [b_rtt], [b_rtt])
                act(rt[:, 10:11], rt[:, 9:10], AF.Exp, [b_rtt], [b_rtt])
                ts1("dve", rt[:, 11:12], rt[:, 10:11], 1.0, ALU.add, [b_rtt], [b_rtt])
                fw.op("dve", lambda e: e.reciprocal(out=rt[:, 12:13], in_=rt[:, 11:12]), [b_rtt], [b_rtt])
                tt("dve", rt[:, 13:14], rt[:, 12:13], rt[:, 6:7], ALU.mult, [b_rtt], [b_rtt])
                tt("dve", rt[:, 14:15], rt[:, 13:14], rt[:, 10:11], ALU.mult, [b_rtt], [b_rtt])
                tt("dve", MF[:, i, :], oh1, oh2, ALU.add, [b_oh], [b_MF])
                cp("dve", MB[:, i, :], MF[:, i, :], [b_MF], [b_MB])
                cp("dve", RB[:, i, :, 3], MF[:, i, :], [b_MF], [b_RALL])
                fw.op("pool", (lambda i: lambda e: e.memset(RB[:, i, :, 1], float(i)))(i), (), [b_RALL])
                cp("dve", RB[:, i, :, 2], oh2, [b_oh], [b_RALL])
                ts1("dve", wtmp[:, 0, :], oh1, rt[:, 13:14], ALU.mult, [b_oh, b_rtt], [b_wtmp])
                stt(wtmp[:, 0, :], oh2, rt[:, 14:15], wtmp[:, 0, :], ALU.mult, ALU.add, [b_oh, b_rtt, b_wtmp], [b_wtmp])
                cp("dve", whb, wtmp[:, 0, :], [b_wtmp], [b_wtmp])
                cp("dve", wtmp[:, 1, :], whb, [b_wtmp], [b_wtmp])
                tt("dve", wtmp[:, 2, :], wtmp[:, 0, :], wtmp[:, 1, :], ALU.subtract, [b_wtmp], [b_wtmp])
                cp("dve", RB[:, i, :, 4], whb, [b_wtmp], [b_RALL])
                cp("dve", RB[:, i, :, 5], wtmp[:, 2, :], [b_wtmp], [b_RALL])
            for i in range(NT):
                pp_, bpp = psum()
                mm(pp_[:, 0:NEXP], tri01b, MB[:, i, :], True, i == 0, [b_trib, b_MB], [bpp])
                for i2 in range(i):
                    mm(pp_[:, 0:NEXP], onesb, MB[:, i2, :], False, i2 == i - 1, [b_trib, b_MB], [bpp])
                cp("act", POS[:, i, :], pp_[:, 0:NEXP], [bpp], [b_POS])
            for e in range(NEXP):
                p = e % 2
                if e + 1 < NEXP:
                    load_w(e + 1, 1 - p)
                for i in range(NT):
                    ts("dve", Sel[:, i, :], C("iota"), POS[:, i, e:e + 1], MF[:, i, e:e + 1], ALU.is_equal, ALU.mult,
                       [b_cst, b_POS, b_MF], [b_Sel])
                pi_ = [psum(), psum()]
                for sc in range(2):
                    for i in range(NT):
                        mm(pi_[sc][0][:, 0:6], Sel[:, i, sc * 128:(sc + 1) * 128], RB[:, i, e, :], i == 0, i == NT - 1,
                           [b_Sel, b_RALL], [pi_[sc][1]])
                    cp("act", idxf[:, sc, :], pi_[sc][0][:, 0:6], [pi_[sc][1]], [b_idxf])
                stt(gf_, idxf[:, :, 1], 128.0, idxf[:, :, 0], ALU.mult, ALU.add, [b_idxf], [b_gidx])
                cp("dve", gidx, gf_, [b_gidx], [b_gidx])
                ts("dve", dstf, idxf[:, :, 3], -1.0, -1.0, ALU.add, ALU.mult, [b_idxf], [b_dsti])
                tt("dve", dstf, dstf, C("trash"), ALU.mult, [b_dsti, b_cst], [b_dsti])
                tt("dve", dstf, dstf, gf_, ALU.add, [b_dsti, b_gidx], [b_dsti])
                stt(dstf, idxf[:, :, 2], float(SEG), dstf, ALU.mult, ALU.add, [b_idxf, b_dsti], [b_dsti])
                cp("dve", dsti, dstf, [b_dsti], [b_dsti])
                tt("dve", wsl, idxf[:, :, 4], idxf[:, :, 5], ALU.add, [b_idxf], [b_dsti])
                for sc in range(2):
                    fw.dma("pool", (lambda sc: lambda en: en.indirect_dma_start(
                        out=xg[:, sc, :], out_offset=None, in_=s_xn2[:, :],
                        in_offset=bass.IndirectOffsetOnAxis(ap=gidx[:, sc:sc + 1], axis=0)))(sc), b_xg[sc], [b_gidx, b_sxn2])
                for sc in range(2):
                    pt, bp = psum(); ptb = pt[:].bitcast(BF16)
                    for kc in range(8):
                        tr(ptb[:, kc * 128:(kc + 1) * 128], xg[:, sc, kc * 128:(kc + 1) * 128], identb, [b_xg[sc], b_idb], [bp])
                    cp("act" if sc == 0 else "dve", xgT[:, :, sc * 128:(sc + 1) * 128], ptb[:, 0:1024].rearrange("p (a b) -> p a b", a=8), [bp], [b_xgT])
                for hc in range(4):
                    pgu, bgu = psum()
                    for kc in range(8):
                        mm(pgu[:, 0:CAP], Wg[p][:, kc, hc * 128:(hc + 1) * 128], xgT[:, kc, :], kc == 0, kc == 7, [b_Wg[p], b_xgT], [bgu])
                    for kc in range(8):
                        mm(pgu[:, CAP:2 * CAP], Wu[p][:, kc, hc * 128:(hc + 1) * 128], xgT[:, kc, :], kc == 0, kc == 7, [b_Wu[p], b_xgT], [bgu])
                    act(sgt, pgu[:, 0:CAP], AF.Silu, [bgu], [b_sgt])
                    tt("dve", hidT[:, hc, :], sgt, pgu[:, CAP:2 * CAP], ALU.mult, [b_sgt, bgu], [b_hidT])
                for sc in range(2):
                    for dh in range(2):
                        pd_, bd_ = psum()
                        for hc in range(4):
                            mm(pd_[:, :], hidT[:, hc, sc * 128:(sc + 1) * 128], Wd[p][:, hc, dh * 512:(dh + 1) * 512], hc == 0, hc == 3,
                               [b_hidT, b_Wd[p]], [bd_])
                        if dh == 0:
                            act(yout[:, sc, 0:512], pd_[:, :], AF.Copy, [bd_, b_dsti], [b_yout], scale=wsl[:, sc:sc + 1])
                        else:
                            ts1("dve", yout[:, sc, 512:1024], pd_[:, :], wsl[:, sc:sc + 1], ALU.mult, [bd_, b_dsti], [b_yout])
                for sc in range(2):
                    fw.dma("pool", (lambda sc: lambda en: en.indirect_dma_start(
                        out=s_yd[:, :], out_offset=bass.IndirectOffsetOnAxis(ap=dsti[:, sc:sc + 1], axis=0),
                        in_=yout[:, sc, :], in_offset=None))(sc), b_yd, [b_dsti, b_yout])
            fw.barrier()
            ar.off = mark_moe
            yl = [[ar.alloc([D], F32) for _ in range(2)] for _ in range(2)]
            b_yl = [[fw.buf("yl%d%d" % (a, b)) for b in range(2)] for a in range(2)]
            for i in range(NT):
                par = i % 2
                for k in range(2):
                    dma("sp", yl[par][k], s_yd[k * SEG + i * 128:k * SEG + (i + 1) * 128, :], b_yl[par][k], [b_yd])
                tt("dve", h1[:, i, :], h1[:, i, :], yl[par][0], ALU.add, [b_h1[i], b_yl[par][0]], [b_h1[i]])
                tt("pool", h1[:, i, :], h1[:, i, :], yl[par][1], ALU.add, [b_h1[i], b_yl[par][1]], [b_h1[i]])
        fx = [ar.alloc([D], F32) for _ in range(2)]; b_fx = [fw.buf("fx0"), fw.buf("fx1")]
        fs = ar.alloc([D], F32); b_fs = fw.buf("fs")
        fst = ar.alloc([8], F32); b_fst = fw.buf("fst")
        for i in range(NT):
            par = i % 2
            act(fs, h1[:, i, :], AF.Square, [b_h1[i]], [b_fs])
            rsum(fst[:, 0:1], fs, [b_fs], [b_fst])
            act(fst[:, 1:2], fst[:, 0:1], AF.Sqrt, [b_fst], [b_fst], bias=1e-6, scale=1.0 / D)
            fw.op("dve", lambda e: e.reciprocal(out=fst[:, 2:3], in_=fst[:, 1:2]), [b_fst], [b_fst])
            stt(fx[par], h1[:, i, :], fst[:, 2:3], ROW("gf"), ALU.mult, ALU.mult, [b_h1[i], b_fst, b_row], [b_fx[par]])
            dma("sp", d_out[i * 128:(i + 1) * 128, :], fx[par], b_out, [b_fx[par]])
        fw.wait_all("sp", [b_out] + ([b_dbg] if dbg else []))
        fw.finish()
    return nc


def make_in_maps(inp):
    x = np.asarray(inp["x"], np.float32)
    cst = build_cst()
    pos = np.arange(8192, dtype=np.float32)
    inv = (10000.0 ** (-np.arange(64, dtype=np.float32) / 64)).astype(np.float32)
    ang = (pos[:, None] * inv[None, :]).astype(np.float32)
    cosf = np.tile(np.cos(ang).astype(np.float32), (1, 4))
    sinf = np.tile(np.sin(ang).astype(np.float32), (1, 4))
    rowp = np.zeros((1, NROW), np.float32)
    rowp[0, 0:1024] = inp["norm1_gain"][0]; rowp[0, 1024:2048] = inp["norm2_gain"][0]; rowp[0, 2048:3072] = inp["final_norm_gain"]
    rowp[0, 3072:3584] = inp["ret_gn_gain"][0]; rowp[0, 3584:4096] = inp["rwkv_gn_gain"][0]
    rowp[0, 4096:4100] = inp["b_route_group"][0]; rowp[0, 4100:4132] = inp["b_route_expert"][0]
    ft = np.zeros((128, NFT), np.float32)
    ft[:, FT_MU:FT_MU + 14] = inp["rwkv_mu"][0].reshape(14, 128).T
    for col, nm in ((FT_W0, "rwkv_w0"), (FT_A0, "rwkv_a0"), (FT_KK, "rwkv_k_k"), (FT_KA, "rwkv_k_a")):
        ft[:, col:col + 4] = inp[nm][0].reshape(4, 128).T
    ft[:, FT_RK:FT_RK + 4] = inp["rwkv_r_k"][0].reshape(4, 128).T
    lora = np.concatenate([inp["rwkv_w_up"][0], inp["rwkv_a_up"][0]], 0).astype(np.float32)
    wr = np.concatenate([inp["w_route_group"][0], inp["w_route_expert"][0]], 1).astype(np.float32)
    maps = []
    for c in range(NCORE):
        b, j = c // 4, c % 4
        xe = np.zeros((SEG + 1, D), np.float32)
        xe[1:] = x[b, j * SEG:(j + 1) * SEG]
        if j > 0:
            xe[0] = x[b, j * SEG - 1]
        pc = np.zeros((128, NPC), np.float32)
        for r in range(NCORE):
            rb, rj = r // 4, r % 4
            if rb == b and rj < j:
                for h in range(4):
                    pc[:, r * 4 + h] = GAM[h] ** (float(SEG) * (j - 1 - rj))
            if rb == b and rj == j - 1:
                pc[:, 32 + r] = 1.0
        maps.append({
            "x_ext": xe, "w_in": np.ascontiguousarray(inp["w_in"][0], np.float32), "cst": cst, "pc": pc, "rowp": rowp, "ftab": ft,
            "lora": lora, "gup": np.ascontiguousarray(inp["rwkv_g_up"][0], np.float32),
            "w_out": np.ascontiguousarray(inp["w_out"][0], np.float32), "wr": wr,
            "w_gate": np.ascontiguousarray(inp["w_gate"][0], np.float32), "w_up": np.ascontiguousarray(inp["w_up"][0], np.float32),
            "w_down": np.ascontiguousarray(inp["w_down"][0], np.float32),
            "cos4": np.ascontiguousarray(cosf[j * SEG:(j + 1) * SEG]), "sin4": np.ascontiguousarray(sinf[j * SEG:(j + 1) * SEG]),
        })
    return maps


_NC_CACHE = {}


def kernel(**inputs):
    if "nc" not in _NC_CACHE:
        _NC_CACHE["nc"] = build_program()
    nc = _NC_CACHE["nc"]
    maps = make_in_maps(inputs)
    res = run_bass_kernel_spmd(nc, maps, core_ids=list(range(NCORE)))
    out = np.zeros((2, 8192, D), np.float32)
    for c in range(NCORE):
        b, j = c // 4, c % 4
        out[b, j * SEG:(j + 1) * SEG] = res.results[c]["out"]
    return out
```
